# Optimizing a Trainium2 kernel written in Bass

```python
import math
import jax
import jax.numpy as jnp
from jax import lax
import numpy as np

D_MODEL = 4096
BATCH = 1
SEQ = 8192
DEPTH = 4

N_BRANCH = 4
BRANCH_WIDTH = D_MODEL // 4
HGRN_HEAD_DIM = 128
HGRN_HEADS = BRANCH_WIDTH // HGRN_HEAD_DIM
HGRN_WIDTH = HGRN_HEADS * HGRN_HEAD_DIM
HGRN_CHUNK = 64
ATTN_HEAD_DIM = 64
ATTN_Q_HEADS = BRANCH_WIDTH // ATTN_HEAD_DIM
ATTN_KV_HEADS = ATTN_Q_HEADS // 4
ATTN_WINDOW = 128
ATTN_BLOCK = 128
SSD_HEAD_DIM = 64
SSD_WIDTH = BRANCH_WIDTH
SSD_HEADS = SSD_WIDTH // SSD_HEAD_DIM
SSD_GROUPS = 4
SSD_STATE = 128
SSD_CONV = 4
SSD_CHUNK = 128
SSD_XBC_WIDTH = SSD_WIDTH + 2 * SSD_GROUPS * SSD_STATE
RET_V_DIM = 128
RET_QK_DIM = 64
RET_HEADS = BRANCH_WIDTH // RET_V_DIM
RET_CHUNK = 128
GATE_RANK = 256
FFN_HIDDEN = -(-8 * D_MODEL // (3 * 256)) * 256

SPLIT_SIZES = (
    HGRN_WIDTH, HGRN_WIDTH, HGRN_WIDTH, HGRN_WIDTH,
    ATTN_Q_HEADS * ATTN_HEAD_DIM, ATTN_KV_HEADS * ATTN_HEAD_DIM,
    ATTN_KV_HEADS * ATTN_HEAD_DIM,
    SSD_WIDTH, SSD_XBC_WIDTH, SSD_HEADS,
    RET_HEADS * RET_QK_DIM, RET_HEADS * RET_QK_DIM,
    RET_HEADS * RET_V_DIM, RET_HEADS * RET_V_DIM,
    GATE_RANK,
)
IN_WIDTH = sum(SPLIT_SIZES)

kernel_name = 'hybrid_gated_parallel_mixer'


def split_columns(a, sizes):
    parts = []
    start = 0
    for size in sizes:
        parts.append(a[..., start:start + size])
        start += size
    return parts


def rms_norm(x, g, eps=1e-6):
    xf = x.astype(jnp.float32)
    y = xf * lax.rsqrt(jnp.mean(xf * xf, axis=-1, keepdims=True) + eps)
    return (y * g.astype(jnp.float32)).astype(x.dtype)


def head_group_norm(x, eps=1e-5):
    xf = x.astype(jnp.float32)
    xc = xf - jnp.mean(xf, axis=-1, keepdims=True)
    var = jnp.mean(xc * xc, axis=-1, keepdims=True)
    return (xc * lax.rsqrt(var + eps)).astype(x.dtype)


def alibi_slopes(n_heads):
    return 2.0 ** (-8.0 * jnp.arange(1, n_heads + 1, dtype=jnp.float32) / n_heads)


def chunked_decay_recurrence(q, k, v, log_decay, chunk):
    out_dtype = v.dtype
    b_, t_, h_, dk = q.shape
    dv = v.shape[-1]
    n = t_ // chunk

    def to_chunks(a):
        return a.astype(jnp.float32).reshape(b_, n, chunk, h_, a.shape[-1]).transpose(1, 0, 3, 2, 4)

    qc, kc, vc = to_chunks(q), to_chunks(k), to_chunks(v)
    bc = jnp.cumsum(to_chunks(log_decay), axis=3)
    causal = jnp.tril(jnp.ones((chunk, chunk), dtype=bool))
    per_channel = log_decay.shape[-1] != 1

    def step(state, inp):
        qi, ki, vi, bi = inp
        b_last = bi[:, :, -1:, :]
        if per_channel:
            diff = bi[:, :, :, None, :] - bi[:, :, None, :, :]
            w = jnp.exp(jnp.where(causal[:, :, None], diff, -jnp.inf))
            scores = jnp.einsum('bhtd,bhsd,bhtsd->bhts', qi, ki, w)
        else:
            diff = bi[:, :, :, None, 0] - bi[:, :, None, :, 0]
            w = jnp.exp(jnp.where(causal, diff, -jnp.inf))
            scores = jnp.einsum('bhtd,bhsd->bhts', qi, ki) * w
        o = (jnp.einsum('bhts,bhsv->bhtv', scores, vi)
             + jnp.einsum('bhtd,bhdv->bhtv', qi * jnp.exp(bi), state))
        k_to_end = ki * jnp.exp(b_last - bi)
        state = (jnp.exp(b_last[:, :, 0, :])[..., None] * state
                 + jnp.einsum('bhsd,bhsv->bhdv', k_to_end, vi))
        return state, o

    state0 = jnp.zeros((b_, h_, dk, dv), jnp.float32)
    _, o = lax.scan(step, state0, (qc, kc, vc, bc))
    o = o.transpose(1, 0, 3, 2, 4).reshape(b_, t_, h_, dv)
    return o.astype(out_dtype)


def hgrn2_mixer(q, f, i, g, lower_bound, norm_g):
    b_, t_, _ = q.shape
    shp = (b_, t_, HGRN_HEADS, HGRN_HEAD_DIM)
    forget = lower_bound + (1.0 - lower_bound) * jax.nn.sigmoid(f.astype(jnp.float32))
    o = chunked_decay_recurrence(jax.nn.silu(q).reshape(shp), (1.0 - forget).reshape(shp),
                                 i.reshape(shp), jnp.log(forget).reshape(shp), HGRN_CHUNK)
    o = rms_norm(o, norm_g.reshape(HGRN_HEADS, HGRN_HEAD_DIM)).reshape(b_, t_, HGRN_WIDTH)
    return o * jax.nn.silu(g)


def sliding_window_attention(q, k, v, sinks, slopes):
    b_, t_, _ = q.shape
    n = t_ // ATTN_BLOCK
    grp = ATTN_Q_HEADS // ATTN_KV_HEADS
    qb = q.astype(jnp.float32).reshape(b_, n, ATTN_BLOCK, ATTN_KV_HEADS, grp, ATTN_HEAD_DIM)

    def with_prev(a):
        a = a.astype(jnp.float32).reshape(b_, n, ATTN_BLOCK, ATTN_KV_HEADS, ATTN_HEAD_DIM)
        prev = jnp.concatenate([jnp.zeros_like(a[:, :1]), a[:, :-1]], axis=1)
        return jnp.concatenate([prev, a], axis=2)

    kk, vv = with_prev(k), with_prev(v)
    scores = jnp.einsum('bnqhgd,bnkhd->bnhgqk', qb, kk) * (ATTN_HEAD_DIM ** -0.5)
    dist = (jnp.arange(ATTN_BLOCK)[:, None] + ATTN_BLOCK) - jnp.arange(2 * ATTN_BLOCK)[None, :]
    in_window = (dist >= 0) & (dist < ATTN_WINDOW)
    key_pos = (jnp.arange(n)[:, None] * ATTN_BLOCK + jnp.arange(2 * ATTN_BLOCK)[None, :]
               - ATTN_BLOCK)
    valid = in_window[None] & (key_pos >= 0)[:, None, :]
    alibi = -slopes.astype(jnp.float32).reshape(ATTN_KV_HEADS, grp)[:, :, None, None] * dist.astype(jnp.float32)
    logits = jnp.where(valid[None, :, None, None], scores + alibi[None, None], -jnp.inf)
    sink = jnp.broadcast_to(sinks.astype(jnp.float32).reshape(ATTN_KV_HEADS, grp)[None, None, :, :, None, None],
                            logits.shape[:-1] + (1,))
    probs = jax.nn.softmax(jnp.concatenate([logits, sink], axis=-1), axis=-1)[..., :-1]
    out = jnp.einsum('bnhgqk,bnkhd->bnqhgd', probs, vv)
    return out.reshape(b_, t_, ATTN_Q_HEADS * ATTN_HEAD_DIM).astype(q.dtype)


def causal_depthwise_conv(x, w, b):
    ch = x.shape[-1]
    y = lax.conv_general_dilated(x, w[:, None, :].astype(x.dtype), window_strides=(1,),
                                 padding=[(SSD_CONV - 1, 0)],
                                 dimension_numbers=('NWC', 'WIO', 'NWC'),
                                 feature_group_count=ch)
    return y + b.astype(x.dtype)


def ssd_mixer(z, xbc, dt, conv_w, conv_b, dt_bias, a_log, d_skip, norm_g):
    b_, t_, _ = z.shape
    xbc = jax.nn.silu(causal_depthwise_conv(xbc, conv_w, conv_b))
    xs, bm, cm = split_columns(xbc, (SSD_WIDTH, SSD_GROUPS * SSD_STATE, SSD_GROUPS * SSD_STATE))
    dt = jax.nn.softplus(dt.astype(jnp.float32) + dt_bias.astype(jnp.float32))
    a = -jnp.exp(a_log.astype(jnp.float32))
    xs = xs.reshape(b_, t_, SSD_HEADS, SSD_HEAD_DIM)
    rep = SSD_HEADS // SSD_GROUPS
    bm = jnp.repeat(bm.reshape(b_, t_, SSD_GROUPS, SSD_STATE), rep, axis=2)
    cm = jnp.repeat(cm.reshape(b_, t_, SSD_GROUPS, SSD_STATE), rep, axis=2)
    y = chunked_decay_recurrence(cm, bm, xs * dt[..., None], (dt * a)[..., None], SSD_CHUNK)
    y = y + d_skip.astype(y.dtype)[:, None] * xs
    y = y.reshape(b_, t_, SSD_WIDTH)
    return rms_norm(y * jax.nn.silu(z), norm_g)


def retention_mixer(q, k, v, g):
    b_, t_, _ = q.shape
    log_gamma = jnp.log(1.0 - 2.0 ** (-5.0 - jnp.arange(RET_HEADS, dtype=jnp.float32)))
    log_decay = jnp.broadcast_to(log_gamma[:, None], (b_, t_, RET_HEADS, 1))
    o = chunked_decay_recurrence(q.reshape(b_, t_, RET_HEADS, RET_QK_DIM),
                                 k.reshape(b_, t_, RET_HEADS, RET_QK_DIM) * (RET_QK_DIM ** -0.5),
                                 v.reshape(b_, t_, RET_HEADS, RET_V_DIM), log_decay, RET_CHUNK)
    o = head_group_norm(o).reshape(b_, t_, RET_HEADS * RET_V_DIM)
    return o * jax.nn.silu(g)


def setup_inputs(seed: int = 0) -> dict:
    key = jax.random.key(seed)
    ks = jax.random.split(key, 22)
    f32 = jnp.float32
    L, D, F = DEPTH, D_MODEL, FFN_HIDDEN

    def normal(k, shape, scale):
        return jax.random.normal(k, shape, f32) * scale

    def gain(k, shape):
        return 1.0 + 0.05 * jax.random.normal(k, shape, f32)

    dt0 = jnp.exp(jax.random.uniform(ks[10], (L, SSD_HEADS), f32, math.log(1e-3), math.log(1e-1)))
    return {
        'x': normal(ks[0], (BATCH, SEQ, D), 1.0),
        'norm_mix_pre': gain(ks[1], (L, D)),
        'norm_mix_post': gain(ks[2], (L, D)),
        'norm_ffn_pre': gain(ks[3], (L, D)),
        'norm_ffn_post': gain(ks[4], (L, D)),
        'w_in': normal(ks[5], (L, D, IN_WIDTH), D ** -0.5),
        'hgrn_lb_logits': normal(ks[6], (L, HGRN_WIDTH), 0.3),
        'hgrn_norm': gain(ks[7], (L, HGRN_WIDTH)),
        'attn_sinks': normal(ks[8], (L, ATTN_Q_HEADS), 0.5),
        'ssd_conv_w': normal(ks[9], (L, SSD_CONV, SSD_XBC_WIDTH), SSD_CONV ** -0.5),
        'ssd_conv_b': normal(ks[11], (L, SSD_XBC_WIDTH), 0.02),
        'ssd_dt_bias': dt0 + jnp.log(-jnp.expm1(-dt0)),
        'ssd_a_log': jnp.log(jax.random.uniform(ks[12], (L, SSD_HEADS), f32, 1.0, 16.0)),
        'ssd_d': 1.0 + 0.1 * jax.random.normal(ks[13], (L, SSD_HEADS), f32),
        'ssd_norm': gain(ks[14], (L, SSD_WIDTH)),
        'w_gate_up': normal(ks[15], (L, GATE_RANK, N_BRANCH * D), GATE_RANK ** -0.5),
        'b_gate': normal(ks[16], (L, N_BRANCH * D), 0.1),
        'w_branch': normal(ks[17], (L, N_BRANCH, BRANCH_WIDTH, D), BRANCH_WIDTH ** -0.5),
        'w_out': normal(ks[18], (L, D, D), D ** -0.5),
        'w_ffn_gate': normal(ks[19], (L, D, F), D ** -0.5),
        'w_ffn_up': normal(ks[20], (L, D, F), D ** -0.5),
        'w_ffn_down': normal(ks[21], (L, F, D), F ** -0.5),
    }


def reference(x, norm_mix_pre, norm_mix_post, norm_ffn_pre, norm_ffn_post, w_in,
              hgrn_lb_logits, hgrn_norm, attn_sinks, ssd_conv_w, ssd_conv_b, ssd_dt_bias,
              ssd_a_log, ssd_d, ssd_norm, w_gate_up, b_gate, w_branch, w_out,
              w_ffn_gate, w_ffn_up, w_ffn_down):
    b_, t_, d_ = x.shape
    p = jax.nn.softmax(hgrn_lb_logits.astype(jnp.float32), axis=0)
    lower_bounds = jnp.cumsum(p, axis=0) - p[0]
    slopes = alibi_slopes(ATTN_Q_HEADS)
    for l in range(DEPTH):
        h = rms_norm(x, norm_mix_pre[l])
        proj = h @ w_in[l]
        (hq, hf, hi, hg, aq, ak, av, sz, sxbc, sdt,
         rq, rk, rv, rg, gate_code) = split_columns(proj, SPLIT_SIZES)
        outs = (
            hgrn2_mixer(hq, hf, hi, hg, lower_bounds[l], hgrn_norm[l]),
            sliding_window_attention(aq, ak, av, attn_sinks[l], slopes),
            ssd_mixer(sz, sxbc, sdt, ssd_conv_w[l], ssd_conv_b[l], ssd_dt_bias[l],
                      ssd_a_log[l], ssd_d[l], ssd_norm[l]),
            retention_mixer(rq, rk, rv, rg),
        )
        gates = jax.nn.sigmoid((gate_code @ w_gate_up[l] + b_gate[l]).astype(jnp.float32))
        gates = gates.reshape(b_, t_, N_BRANCH, d_).astype(x.dtype)
        merged = gates[:, :, 0, :] * (outs[0].astype(x.dtype) @ w_branch[l, 0])
        for br in range(1, N_BRANCH):
            merged = merged + gates[:, :, br, :] * (outs[br].astype(x.dtype) @ w_branch[l, br])
        x = x + rms_norm(merged @ w_out[l], norm_mix_post[l])
        h = rms_norm(x, norm_ffn_pre[l])
        ff = (jax.nn.silu(h @ w_ffn_gate[l]) * (h @ w_ffn_up[l])) @ w_ffn_down[l]
        x = x + rms_norm(ff, norm_ffn_post[l])
    return x
```

```python
import math
from contextlib import ExitStack, contextmanager

import numpy as np
import concourse.bass as bass
import concourse.mybir as mybir
from concourse.bass_utils import run_bass_kernel_spmd

F32 = mybir.dt.float32
BF16 = mybir.dt.bfloat16
ALU = mybir.AluOpType
AF = mybir.ActivationFunctionType
AX = mybir.AxisListType

TB = 512
NEG = -30000.0


class Res:
    __slots__ = ("w", "r", "x")

    def __init__(self, x=False):
        self.w = None
        self.r = {}
        self.x = x


class FW:
    NDS = 16

    def __init__(self, nc):
        self.nc = nc
        self.eng = {"pe": nc.tensor, "act": nc.scalar, "dve": nc.vector, "pool": nc.gpsimd, "sp": nc.sync}
        self.ops = {k: [] for k in self.eng}
        self.cnt = {k: 0 for k in ("pe", "act", "dve", "pool")}
        self.sem = {k: nc.alloc_semaphore(name=f"sem_{k}") for k in ("pe", "act", "dve", "pool")}
        self.waited = {k: {} for k in self.eng}
        self.dsem, self.dval, self.dnext = {}, {}, {}
        for q in ("sp", "pool", "act"):
            self.dsem[q] = [nc.alloc_semaphore(name=f"dsem_{q}{i}") for i in range(self.NDS)]
            self.dval[q] = [0] * self.NDS
            self.dnext[q] = 0
        self.nops = 0

    def _semof(self, key):
        return self.sem[key] if isinstance(key, str) else self.dsem[key[0]][key[1]]

    def _need(self, e, ev):
        if ev is None:
            return
        key, val = ev
        if key == e and e == "pe":
            return
        if self.waited[e].get(key, 0) >= val:
            return
        self.waited[e][key] = val
        s = self._semof(key)
        self.ops[e].append(lambda h, s=s, val=val: h.wait_ge(s, val))

    def _deps(self, e, reads, writes):
        for r in reads:
            self._need(e, r.w)
        for w in writes:
            self._need(e, w.w)
            for k, v in w.r.items():
                self._need(e, (k, v))

    def _commit(self, ev, reads, writes):
        k, v = ev
        for r in reads:
            if r.r.get(k, 0) < v:
                r.r[k] = v
        for w in writes:
            w.w = ev
            w.r = {}

    def op(self, e, fn, reads=(), writes=()):
        if any(r.x for r in reads):
            writes = list(writes) + [r for r in reads if r.x]
            reads = [r for r in reads if not r.x]
        self._deps(e, reads, writes)
        self.cnt[e] += 1
        s = self.sem[e]
        self.ops[e].append(lambda h, fn=fn, s=s: fn(h).then_inc(s, 1))
        self._commit((e, self.cnt[e]), reads, writes)
        self.nops += 1

    def dma(self, q, out, in_, reads=(), writes=()):
        self._deps(q, reads, writes)
        i = self.dnext[q]
        self.dnext[q] = (i + 1) % self.NDS
        key = (q, i)
        if self.dval[q][i] > 0:
            self._need(q, (key, self.dval[q][i]))
        self.dval[q][i] += 16
        s = self.dsem[q][i]
        self.ops[q].append(lambda h, out=out, in_=in_, s=s: h.dma_start(out=out, in_=in_).then_inc(s, 16))
        self._commit((key, self.dval[q][i]), reads, writes)
        self.nops += 1

    def barrier(self):
        for e in self.eng:
            for k in self.cnt:
                if self.cnt[k] > 0:
                    self._need(e, (k, self.cnt[k]))
            for q in self.dsem:
                for i in range(self.NDS):
                    if self.dval[q][i] > 0:
                        self._need(e, ((q, i), self.dval[q][i]))

    def emit(self):
        nc = self.nc
        with nc.Block() as block:
            @block.tensor
            def _(h):
                for f in self.ops["pe"]:
                    f(h)

            @block.scalar
            def _(h):
                for f in self.ops["act"]:
                    f(h)

            @block.vector
            def _(h):
                for f in self.ops["dve"]:
                    f(h)

            @block.gpsimd
            def _(h):
                for f in self.ops["pool"]:
                    f(h)

            @block.sync
            def _(h):
                for f in self.ops["sp"]:
                    f(h)


class Tile:
    __slots__ = ("t", "res")

    def __init__(self, t):
        self.t = t
        self.res = Res()


class Ring:
    def __init__(self, tiles):
        self.tiles = tiles
        self.i = 0

    def next(self):
        t = self.tiles[self.i]
        self.i = (self.i + 1) % len(self.tiles)
        return t


class Cfg:
    def __init__(s, D, T, L):
        s.D, s.T, s.L = D, T, L
        s.DC = D // 128
        s.BW = D // 4
        s.BC = s.BW // 128
        s.HG = s.BW // 128
        s.AQ = s.BW // 64
        s.AKV = s.AQ // 4
        s.SH = s.BW // 64
        s.SG = 4
        s.REP = s.SH // s.SG
        s.XBC = s.BW + 2 * s.SG * 128
        s.XC = s.XBC // 128
        s.RH = s.BW // 128
        s.F = -(-8 * D // (3 * 256)) * 256
        s.FC = s.F // 128
        names = ['hq', 'hf', 'hi', 'hg', 'aq', 'ak', 'av', 'sz', 'sxbc', 'sdt', 'rq', 'rk', 'rv', 'rg', 'gc']
        sizes = [s.BW] * 4 + [s.AQ * 64, s.AKV * 64, s.AKV * 64, s.BW, s.XBC, s.SH,
                              s.RH * 64, s.RH * 64, s.RH * 128, s.RH * 128, 256]
        s.off, s.size = {}, {}
        o = 0
        for n, z in zip(names, sizes):
            s.off[n], s.size[n] = o, z
            o += z
        s.INW = o
        fm = ['hq', 'hf', 'aq', 'ak', 'sxbc', 'rq', 'rk', 'gc']
        tm = ['hi', 'hg', 'av', 'sz', 'sdt', 'rk', 'rv', 'rg']
        s.foff, s.toff = {}, {}
        s.panels = []
        o = 0
        for n in fm:
            s.foff[n] = o
            for k in range(0, s.size[n], 128):
                nc_ = min(128, s.size[n] - k)
                s.panels.append(('F', s.off[n] + k, nc_, o + k))
            o += s.size[n]
        s.NF = o
        o = 0
        for n in tm:
            s.toff[n] = o
            for k in range(0, s.size[n], 128):
                nc_ = min(128, s.size[n] - k)
                s.panels.append(('T', s.off[n] + k, nc_, o + k))
            o += s.size[n]
        s.NT = o
        s.NMAX = max(s.INW, 4 * D, s.F)


def make_consts(c):
    i = np.arange(128)
    blocks = {}
    blocks['ident'] = np.eye(128, dtype=np.float32)
    blocks['U'] = (i[:, None] <= i[None, :]).astype(np.float32)
    blocks['M1'] = (i[:, None] > i[None, :]).astype(np.float32)
    blocks['NEGM'] = NEG * (i[:, None] > i[None, :]).astype(np.float32)
    blocks['ones'] = np.ones((128, 128), np.float32)
    gam = 1.0 - 2.0 ** (-5.0 - np.arange(c.RH, dtype=np.float64))
    mt = np.zeros((128, c.RH * 128), np.float64)
    qd = np.zeros((128, c.RH * 128), np.float64)
    kd = np.zeros((128, c.RH), np.float64)
    for h in range(c.RH):
        d = (i[None, :] - i[:, None]).astype(np.float64)
        mt[:, h * 128:(h + 1) * 128] = np.where(d >= 0, gam[h] ** np.maximum(d, 0), 0.0) * (64 ** -0.5)
        qd[:, h * 128:(h + 1) * 128] = (gam[h] ** (i + 1.0))[None, :]
        kd[:, h] = gam[h] ** (127.0 - i) * (64 ** -0.5)
    blocks['ret_mt'] = mt.astype(np.float32)
    blocks['ret_qd'] = qd.astype(np.float32)
    blocks['ret_kd'] = kd.astype(np.float32)
    dist = (i[:, None] + 128) - np.arange(256)[None, :]
    valid = (dist >= 0) & (dist < 128)
    blocks['attn_nd'] = np.where(valid, -dist.astype(np.float64), -1e9).astype(np.float32)
    pm = np.zeros((128, 256), np.float32)
    pm[:, :128] = NEG
    blocks['prevmask'] = pm
    offs, o = {}, 0
    for k, v in blocks.items():
        offs[k] = (o, v.shape[1])
        o += v.shape[1]
    arr = np.concatenate(list(blocks.values()), axis=1).astype(np.float32)
    gam128 = [float(g ** 128.0) for g in gam]
    return arr, offs, gam128


WEIGHT_NAMES = ['norm_mix_pre', 'norm_mix_post', 'norm_ffn_pre', 'norm_ffn_post', 'w_in', 'hgrn_lb_logits',
                'hgrn_norm', 'attn_sinks', 'ssd_conv_w', 'ssd_conv_b', 'ssd_dt_bias', 'ssd_a_log', 'ssd_d',
                'ssd_norm', 'w_gate_up', 'b_gate', 'w_branch', 'w_out', 'w_ffn_gate', 'w_ffn_up', 'w_ffn_down']


class Builder:
    def __init__(self, cfg, debug=()):
        c = self.c = cfg
        nc = self.nc = bass.Bass("TRN2", target_bir_lowering=False)
        self.fw = FW(nc)
        self.carr, self.coff, self.gam128 = make_consts(c)
        self.debug = debug
        L = c.L

        def din(name, shape):
            return nc.dram_tensor(name, list(shape), F32, kind="ExternalInput").ap()

        def dint(name, shape, dt):
            return nc.dram_tensor(name, list(shape), dt, kind="Internal").ap()

        self.x = din("x", [c.T, c.D])
        self.consts_d = din("consts", list(self.carr.shape))
        shapes = {
            'norm_mix_pre': [L, c.D], 'norm_mix_post': [L, c.D], 'norm_ffn_pre': [L, c.D], 'norm_ffn_post': [L, c.D],
            'w_in': [L, c.D, c.INW], 'hgrn_lb_logits': [L, c.BW], 'hgrn_norm': [L, c.BW], 'attn_sinks': [L, c.AQ],
            'ssd_conv_w': [L, 4, c.XBC], 'ssd_conv_b': [L, c.XBC], 'ssd_dt_bias': [L, c.SH], 'ssd_a_log': [L, c.SH],
            'ssd_d': [L, c.SH], 'ssd_norm': [L, c.BW], 'w_gate_up': [L, 256, 4 * c.D], 'b_gate': [L, 4 * c.D],
            'w_branch': [L, 4, c.BW, c.D], 'w_out': [L, c.D, c.D], 'w_ffn_gate': [L, c.D, c.F],
            'w_ffn_up': [L, c.D, c.F], 'w_ffn_down': [L, c.F, c.D],
        }
        self.shapes = shapes
        self.w = {n: din(n, shapes[n]) for n in WEIGHT_NAMES}
        self.y = nc.dram_tensor("y", [c.T, c.D], F32, kind="ExternalOutput").ap()
        self.dbg_out = {}
        self.xT = dint("xT", [c.D, c.T], F32)
        self.ybuf = dint("ybuf", [c.D, c.T], F32)
        self.projF = dint("projF", [c.NF, c.T], F32)
        self.projT = dint("projT", [c.T, c.NT], F32)
        self.outsT = dint("outsT", [c.D, c.T], BF16)
        self.wp = []
        for l in range(L):
            self.wp.append({
                'in': dint(f"wp_in{l}", [len(c.panels), 128, c.DC * 128], BF16),
                'gu': dint(f"wp_gu{l}", [4 * c.DC, 128, 2 * 128], BF16),
                'br': dint(f"wp_br{l}", [4 * c.DC, 128, c.BC * 128], BF16),
                'out': dint(f"wp_out{l}", [c.DC, 128, c.DC * 128], BF16),
                'fg': dint(f"wp_fg{l}", [c.FC, 128, c.DC * 128], BF16),
                'fu': dint(f"wp_fu{l}", [c.FC, 128, c.DC * 128], BF16),
                'fd': dint(f"wp_fd{l}", [c.DC, 128, c.FC * 128], BF16),
            })
        self.base = ExitStack()
        P = self.psb
        self.cst = {}
        for k in ('ident', 'U', 'M1', 'NEGM', 'ones'):
            self.cst[k] = P(f"c_{k}", [128, 128], F32)
        self.identb = P("identb", [128, 128], BF16)
        self.gains = P("gains", [128, 4 * c.DC], F32)
        self.bgate = P("bgate", [128, 4 * c.DC], F32)
        self.lb = P("lb", [128, L * c.HG], F32)
        self.oml = P("oml", [128, L * c.HG], F32)
        self.pring = Ring([Tile(self.base.enter_context(nc.psum_tensor(f"pacc{i}", [128, 512], F32))) for i in range(6)])
        self.pss = Tile(self.base.enter_context(nc.psum_tensor("pss", [128, 512], F32)))
        self.ptb = Tile(self.base.enter_context(nc.psum_tensor("ptb", [128, 1024], BF16)))
        for t in self.pring.tiles + [self.pss, self.ptb]:
            t.res.x = True
        self.evac_i = 0

    def psb(self, name, shape, dt):
        return Tile(self.base.enter_context(self.nc.sbuf_tensor(name, list(shape), dt)))

    @contextmanager
    def scope(self):
        st = ExitStack()

        def alloc(name, shape, dt):
            self.uid = getattr(self, "uid", 0) + 1
            return Tile(st.enter_context(self.nc.sbuf_tensor(f"{name}_{self.uid}", list(shape), dt)))
        try:
            yield alloc
        finally:
            self.fw.barrier()
            st.close()

    def tt(self, e, out, a, b, op, R, W):
        self.fw.op(e, lambda h: h.tensor_tensor(out=out, in0=a, in1=b, op=op), R, W)

    def ts(self, e, out, a, s1, s2, op0, op1, R, W):
        if op1 is None:
            self.fw.op(e, lambda h: h.tensor_scalar(out=out, in0=a, scalar1=s1, scalar2=None, op0=op0), R, W)
        else:
            self.fw.op(e, lambda h: h.tensor_scalar(out=out, in0=a, scalar1=s1, scalar2=s2, op0=op0, op1=op1), R, W)

    def stt(self, out, a, scalar, b, op0, op1, R, W):
        self.fw.op("dve", lambda h: h.scalar_tensor_tensor(out=out, in0=a, scalar=scalar, in1=b, op0=op0, op1=op1), R, W)

    def actv(self, out, in_, func, R, W, bias=None, scale=None, accum=None):
        kw = {}
        if bias is not None:
            kw['bias'] = bias
        if scale is not None:
            kw['scale'] = scale
        if accum is not None:
            kw['accum_out'] = accum
        self.fw.op("act", lambda h: h.activation(out=out, in_=in_, func=func, **kw), R, W)

    def cp(self, e, out, in_, R, W):
        if e == "act":
            self.fw.op("act", lambda h: h.copy(out=out, in_=in_), R, W)
        else:
            self.fw.op(e, lambda h: h.tensor_copy(out=out, in_=in_), R, W)

    def evac(self, out, in_, R, W):
        self.evac_i += 1
        self.cp("act" if self.evac_i % 2 else "dve", out, in_, R, W)

    def mm(self, out, lhsT, rhs, start, stop, R, W):
        self.fw.op("pe", lambda h: h.matmul(out, lhsT, rhs, start=start, stop=stop), R, W)

    def tr(self, out, in_, ident, R, W):
        self.fw.op("pe", lambda h: h.transpose(out, in_, ident), R, W)

    def red(self, out, in_, op, R, W):
        self.fw.op("dve", lambda h: h.tensor_reduce(out=out, in_=in_, axis=AX.X, op=op), R, W)

    def scan(self, out, d0, d1, R, W):
        self.fw.op("dve", lambda h: h.tensor_tensor_scan(out=out, data0=d0, data1=d1, initial=0.0, op0=ALU.mult, op1=ALU.add), R, W)

    def recip(self, out, in_, R, W):
        self.fw.op("dve", lambda h: h.reciprocal(out=out, in_=in_), R, W)

    def memset(self, e, ap, val, W):
        self.fw.op(e, lambda h: h.memset(ap, val), (), W)

    def rsqrt_inplace(self, tile_ap, res, mean_scale, eps):
        self.ts("dve", tile_ap, tile_ap, mean_scale, eps, ALU.mult, ALU.add, [res], [res])
        self.actv(tile_ap, tile_ap, AF.Sqrt, [res], [res])
        self.fw.op("dve", lambda h: h.reciprocal(out=tile_ap, in_=tile_ap), [res], [res])

    def bcast_load(self, q, tile, src_row_ap, n):
        src = bass.AP(src_row_ap.tensor, src_row_ap.offset, [[0, 128], [1, n]])
        self.fw.dma(q, tile.t[:, 0:n], src, writes=[tile.res])

    def load_cols(self, S, dst_ap, dst_res, src2d, n):
        tmp = S("lc_tmp", [128, 128], F32)
        self.fw.dma("sp", tmp.t[0:n, :], src2d, writes=[tmp.res])
        ps = self.pring.next()
        self.tr(ps.t[:, 0:n], tmp.t[0:n, :], self.cst['ident'].t[0:n, 0:n], [tmp.res, self.cst['ident'].res], [ps.res])
        self.cp("dve", dst_ap, ps.t[:, 0:n], [ps.res], [dst_res])

    def setup_consts(self):
        c = self.c
        for k in ('ident', 'U', 'M1', 'NEGM', 'ones'):
            o, n = self.coff[k]
            self.fw.dma("sp", self.cst[k].t[:], self.consts_d[:, o:o + n], writes=[self.cst[k].res])
        self.cp("dve", self.identb.t[:], self.cst['ident'].t[:], [self.cst['ident'].res], [self.identb.res])
        L, HG = c.L, c.HG
        with self.scope() as S:
            lg = S("lg", [128, L * HG], F32)
            self.load_cols(S, lg.t[:, :], lg.res,
                           self.w['hgrn_lb_logits'].rearrange("l (h p) -> (l h) p", p=128), L * HG)
            mx = S("lbmx", [128, HG], F32)
            self.cp("dve", mx.t[:], lg.t[:, 0:HG], [lg.res], [mx.res])
            for l in range(1, L):
                self.tt("dve", mx.t[:], mx.t[:], lg.t[:, l * HG:(l + 1) * HG], ALU.max, [mx.res, lg.res], [mx.res])
            for l in range(L):
                self.tt("dve", lg.t[:, l * HG:(l + 1) * HG], lg.t[:, l * HG:(l + 1) * HG], mx.t[:], ALU.subtract,
                        [lg.res, mx.res], [lg.res])
            self.actv(lg.t[:], lg.t[:], AF.Exp, [lg.res], [lg.res])
            sm = S("lbsum", [128, HG], F32)
            self.cp("dve", sm.t[:], lg.t[:, 0:HG], [lg.res], [sm.res])
            for l in range(1, L):
                self.tt("dve", sm.t[:], sm.t[:], lg.t[:, l * HG:(l + 1) * HG], ALU.add, [sm.res, lg.res], [sm.res])
            self.fw.op("dve", lambda h: h.reciprocal(out=sm.t[:], in_=sm.t[:]), [sm.res], [sm.res])
            self.memset("dve", self.lb.t[:, 0:HG], 0.0, [self.lb.res])
            for l in range(1, L):
                pl = S(f"lbp{l}", [128, HG], F32)
                self.tt("dve", pl.t[:], lg.t[:, l * HG:(l + 1) * HG], sm.t[:], ALU.mult, [lg.res, sm.res], [pl.res])
                self.tt("dve", self.lb.t[:, l * HG:(l + 1) * HG], self.lb.t[:, (l - 1) * HG:l * HG], pl.t[:], ALU.add,
                        [self.lb.res, pl.res], [self.lb.res])
            self.ts("dve", self.oml.t[:], self.lb.t[:], -1.0, 1.0, ALU.mult, ALU.add, [self.lb.res], [self.oml.res])

    def convert_matrix(self, S, stage, stageb, Wap, K, N, wp, pbase, panels, KCtot):
        KC = K // 128
        NPAN = len(panels)
        regular = all(nc_ == 128 and c0 == 128 * i for i, (c0, nc_) in enumerate(panels))
        need = max(N, NPAN * 128)
        RC = max(1, min(4, self.conv_cap // need, KC))
        st, sb = stage.next(), stageb.next()
        for cc0 in range(0, KC, RC):
            rc = min(RC, KC - cc0)
            self.fw.dma("sp", st.t[:, 0:rc * N].rearrange("p (r n) -> p r n", n=N),
                        Wap[cc0 * 128:(cc0 + rc) * 128, :].rearrange("(r p) n -> p r n", p=128), writes=[st.res])
            sbv = sb.t[:, 0:NPAN * rc * 128].rearrange("p (j r n) -> p j r n", r=rc, n=128)
            sbm = sb.t[:, 0:NPAN * rc * 128].rearrange("p (j m) -> p j m", m=rc * 128)
            if regular:
                for r in range(rc):
                    for j0 in range(0, NPAN, 16):
                        j1 = min(NPAN, j0 + 16)
                        self.conv_i += 1
                        self.cp("dve" if self.conv_i % 2 else "pool", sbv[:, j0:j1, r, :],
                                st.t[:, r * N + j0 * 128:r * N + j1 * 128].rearrange("p (j n) -> p j n", n=128),
                                [st.res], [sb.res])
                for j0 in range(0, NPAN, 8):
                    j1 = min(NPAN, j0 + 8)
                    dst = wp[pbase + j0:pbase + j1, :, cc0 * 128:(cc0 + rc) * 128].rearrange("j p n -> p j n")
                    self.fw.dma("act", dst, sbm[:, j0:j1, :], reads=[sb.res])
            else:
                stv = st.t[:, 0:rc * N].rearrange("p (r n) -> p r n", n=N)
                for pi, (c0, nc_) in enumerate(panels):
                    self.conv_i += 1
                    self.cp("dve" if self.conv_i % 2 else "pool", sbv[:, pi, :, 0:nc_], stv[:, :, c0:c0 + nc_], [st.res], [sb.res])
                for pi, (c0, nc_) in enumerate(panels):
                    dst = wp[pbase + pi, :, cc0 * 128:(cc0 + rc) * 128]
                    if nc_ == 128:
                        self.fw.dma("act", dst, sbm[:, pi, :], reads=[sb.res])
                    else:
                        self.fw.dma("act", dst.rearrange("p (r n) -> p r n", n=128)[:, :, 0:nc_], sbv[:, pi, :, 0:nc_], reads=[sb.res])

    def convert_layer(self, l):
        c = self.c
        self.conv_i = 0
        with self.scope() as S:
            need = max(c.NMAX, len(c.panels) * 128)
            self.conv_cap = 25600 if need > 6400 else 4 * need
            stage = Ring([S("cst", [128, self.conv_cap], F32)])
            stageb = Ring([S("csb", [128, self.conv_cap], BF16)])
            w, wp = self.w, self.wp[l]
            self.convert_matrix(S, stage, stageb, w['w_in'][l], c.D, c.INW, wp['in'], 0,
                                [(p[1], p[2]) for p in c.panels], c.DC)
            reg = lambda n: [(128 * i, 128) for i in range(n // 128)]
            self.convert_matrix(S, stage, stageb, w['w_gate_up'][l], 256, 4 * c.D, wp['gu'], 0, reg(4 * c.D), 2)
            for br in range(4):
                self.convert_matrix(S, stage, stageb, w['w_branch'][l, br], c.BW, c.D, wp['br'], br * c.DC, reg(c.D), c.BC)
            self.convert_matrix(S, stage, stageb, w['w_out'][l], c.D, c.D, wp['out'], 0, reg(c.D), c.DC)
            self.convert_matrix(S, stage, stageb, w['w_ffn_gate'][l], c.D, c.F, wp['fg'], 0, reg(c.F), c.DC)
            self.convert_matrix(S, stage, stageb, w['w_ffn_up'][l], c.D, c.F, wp['fu'], 0, reg(c.F), c.DC)
            self.convert_matrix(S, stage, stageb, w['w_ffn_down'][l], c.F, c.D, wp['fd'], 0, reg(c.D), c.FC)

    def transpose_in(self):
        c = self.c
        idt = self.cst['ident']
        with self.scope() as S:
            xin = Ring([S(f"xin{i}", [128, c.D], F32) for i in range(2)])
            xo = Ring([S(f"xo{i}", [128, 4, 128], F32) for i in range(3)])
            for i in range(c.T // 128):
                t = xin.next()
                self.fw.dma("sp", t.t[:], self.x[i * 128:(i + 1) * 128, :], writes=[t.res])
                for g in range(c.DC // 4):
                    ps = self.pring.next()
                    for k in range(4):
                        cc = g * 4 + k
                        self.tr(ps.t[:, k * 128:(k + 1) * 128], t.t[:, cc * 128:(cc + 1) * 128], idt.t[:],
                                [t.res, idt.res], [ps.res])
                    o = xo.next()
                    self.evac(o.t[:].rearrange("p a b -> p (a b)"), ps.t[:], [ps.res], [o.res])
                    dst = self.xT[g * 512:(g + 1) * 512, i * 128:(i + 1) * 128].rearrange("(a p) t -> p a t", p=128)
                    self.fw.dma("pool", dst, o.t[:], reads=[o.res])

    def transpose_out(self):
        c = self.c
        idt = self.cst['ident']
        with self.scope() as S:
            xi = Ring([S(f"xoi{i}", [128, c.DC, 128], F32) for i in range(2)])
            yo = Ring([S(f"yo{i}", [128, c.D], F32) for i in range(2)])
            self.out_res = Res()
            for i in range(c.T // 128):
                t = xi.next()
                self.fw.dma("sp", t.t[:], self.xT[:, i * 128:(i + 1) * 128].rearrange("(a p) t -> p a t", p=128),
                            writes=[t.res])
                o = yo.next()
                for g in range(c.DC // 4):
                    ps = self.pring.next()
                    for k in range(4):
                        cc = g * 4 + k
                        self.tr(ps.t[:, k * 128:(k + 1) * 128], t.t[:, cc, :], idt.t[:], [t.res, idt.res], [ps.res])
                    self.evac(o.t[:, g * 512:(g + 1) * 512], ps.t[:], [ps.res], [o.res])
                self.fw.dma("pool", self.y[i * 128:(i + 1) * 128, :], o.t[:], reads=[o.res], writes=[self.out_res])

    def load_layer_params(self, l):
        c = self.c
        with self.scope() as S:
            for gi, n in enumerate(('norm_mix_pre', 'norm_mix_post', 'norm_ffn_pre', 'norm_ffn_post')):
                self.load_cols(S, self.gains.t[:, gi * c.DC:(gi + 1) * c.DC], self.gains.res,
                               self.w[n][l].rearrange("(a p) -> a p", p=128), c.DC)
            self.load_cols(S, self.bgate.t[:, :], self.bgate.res,
                           self.w['b_gate'][l].rearrange("(a p) -> a p", p=128), 4 * c.DC)

    def load_panel(self, wp, pidx, KC):
        pieces = []
        for c0 in range(0, KC, 43):
            n = min(43, KC - c0)
            wt = self.wring.next()
            self.fw.dma("sp", wt.t[:, 0:n * 128], wp[pidx, :, c0 * 128:(c0 + n) * 128], writes=[wt.res])
            pieces.append((wt, c0, n))
        return pieces

    @staticmethod
    def wslice(pieces, cc, ncols):
        wt, c0, n = pieces[cc // 43]
        return wt.t[:, (cc - c0) * 128:(cc - c0) * 128 + ncols], wt.res

    def gemm_F(self, ps_ap, ps_res, pieces, KC, ncols, act, koff=0):
        for cc in range(KC):
            w_ap, w_res = self.wslice(pieces, cc, ncols)
            self.mm(ps_ap, w_ap, act.t[:, koff + cc, :], cc == 0, cc == KC - 1, [w_res, act.res], [ps_res])

    def sumsq_accum(self, src_ap, src_res, j, n, sq):
        self.actv(sq.t[:], src_ap, AF.Square, [src_res], [sq.res])
        one = self.cst['ones']
        self.mm(self.pss.t[:], one.t[:], sq.t[:], j == 0, j == n - 1, [one.res, sq.res], [self.pss.res])

    def rstd_from_pss(self, rstd):
        c = self.c
        self.ts("dve", rstd.t[:], self.pss.t[:], 1.0 / c.D, 1e-6, ALU.mult, ALU.add, [self.pss.res], [rstd.res])
        self.actv(rstd.t[:], rstd.t[:], AF.Sqrt, [rstd.res], [rstd.res])
        self.fw.op("dve", lambda h: h.reciprocal(out=rstd.t[:], in_=rstd.t[:]), [rstd.res], [rstd.res])

    def phase_norm_proj(self, l):
        c = self.c
        wp = self.wp[l]['in']
        with self.scope() as S:
            hT = S("hT", [128, c.DC, TB], BF16)
            self.wring = Ring([S(f"wr{i}", [128, 43 * 128], BF16) for i in range(4)])
            sm = Ring([S(f"sm{i}", [128, TB], F32) for i in range(6)])
            rstd = S("rstd", [128, TB], F32)
            ot = Ring([S(f"ot{i}", [128, TB], F32) for i in range(3)])
            wt4 = Ring([S(f"wt4_{i}", [128, 4, c.DC * 128], BF16) for i in range(2)])
            items, P_, ii = [], c.panels, 0
            while ii < len(P_):
                mode, col0, ncols, dst = P_[ii]
                if mode == 'T' and ii + 3 < len(P_) and all(
                        P_[ii + k][0] == 'T' and P_[ii + k][2] == 128 and P_[ii + k][3] == dst + 128 * k for k in range(4)):
                    items.append(('T4', ii))
                    ii += 4
                else:
                    items.append((mode, ii))
                    ii += 1
            for blk in range(c.T // TB):
                ts_ = slice(blk * TB, (blk + 1) * TB)
                for j in range(c.DC):
                    t = sm.next()
                    self.fw.dma("pool", t.t[:], self.xT[j * 128:(j + 1) * 128, ts_], writes=[t.res])
                    sq = sm.next()
                    self.sumsq_accum(t.t[:], t.res, j, c.DC, sq)
                self.rstd_from_pss(rstd)
                for j in range(c.DC):
                    t = sm.next()
                    self.fw.dma("pool", t.t[:], self.xT[j * 128:(j + 1) * 128, ts_], writes=[t.res])
                    self.stt(hT.t[:, j, :], t.t[:], self.gains.t[:, j:j + 1], rstd.t[:], ALU.mult, ALU.mult,
                             [t.res, self.gains.res, rstd.res], [hT.res])
                for kind, pi in items:
                    mode, col0, ncols, dst = c.panels[pi]
                    if kind == 'T4':
                        w4 = wt4.next()
                        self.fw.dma("sp", w4.t[:], wp[pi:pi + 4, :, :].rearrange("j p n -> p j n"), writes=[w4.res])
                        for tt in range(TB // 128):
                            ps = self.pring.next()
                            o = ot.next()
                            for cc in range(c.DC):
                                self.mm(ps.t[:, :].rearrange("p (j n) -> p j n", n=128), hT.t[:, cc, tt * 128:(tt + 1) * 128],
                                        w4.t[:, :, cc * 128:(cc + 1) * 128], cc == 0, cc == c.DC - 1, [w4.res, hT.res], [ps.res])
                            self.evac(o.t[:, :], ps.t[:, :], [ps.res], [o.res])
                            r0 = blk * TB + tt * 128
                            self.fw.dma("pool", self.projT[r0:r0 + 128, dst:dst + 512], o.t[:, :], reads=[o.res])
                        continue
                    pieces = self.load_panel(wp, pi, c.DC)
                    ps = self.pring.next()
                    o = ot.next()
                    if mode == 'F':
                        self.gemm_F(ps.t[0:ncols, :], ps.res, pieces, c.DC, ncols, hT)
                        self.evac(o.t[0:ncols, :], ps.t[0:ncols, :], [ps.res], [o.res])
                        self.fw.dma("pool", self.projF[dst:dst + ncols, ts_], o.t[0:ncols, :], reads=[o.res])
                    else:
                        for tt in range(TB // 128):
                            for cc in range(c.DC):
                                w_ap, w_res = self.wslice(pieces, cc, ncols)
                                self.mm(ps.t[:, tt * 128:tt * 128 + ncols], hT.t[:, cc, tt * 128:(tt + 1) * 128], w_ap,
                                        cc == 0, cc == c.DC - 1, [w_res, hT.res], [ps.res])
                        pv = ps.t[:].rearrange("p (a n) -> p a n", n=128)[:, :, 0:ncols]
                        ov = o.t[:].rearrange("p (a n) -> p a n", n=128)[:, :, 0:ncols]
                        self.evac(ov, pv, [ps.res], [o.res])
                        dstap = self.projT[ts_, dst:dst + ncols].rearrange("(a p) n -> p a n", p=128)
                        self.fw.dma("pool", dstap, ov, reads=[o.res])

    def phase_merge_ffn(self, l):
        c = self.c
        wp = self.wp[l]
        DC, FC, BC = c.DC, c.FC, c.BC
        G0, G1, G2, G3 = 0, DC, 2 * DC, 3 * DC
        with self.scope() as S:
            actA = S("actA", [128, DC, TB], BF16)
            hid = S("hid", [128, max(FC, DC), TB], BF16)
            self.wring = Ring([S(f"wr{i}", [128, 43 * 128], BF16) for i in range(4)])
            sm = Ring([S(f"sm{i}", [128, TB], F32) for i in range(6)])
            rstd = S("rstd", [128, TB], F32)
            gcf = S("gcf", [128, 2, TB], F32)
            gcb = S("gcb", [128, 2, TB], BF16)
            acc = Ring([S(f"acc{i}", [128, TB], F32) for i in range(2)])
            yres = [Res() for _ in range(DC)]
            xres = [Res() for _ in range(DC)]
            gcoff = c.foff['gc']
            for blk in range(c.T // TB):
                ts_ = slice(blk * TB, (blk + 1) * TB)
                self.fw.dma("sp", hid.t[:, 0:DC, :], self.outsT[:, ts_].rearrange("(a p) t -> p a t", p=128),
                            writes=[hid.res])
                self.fw.dma("sp", gcf.t[:], self.projF[gcoff:gcoff + 256, ts_].rearrange("(a p) t -> p a t", p=128),
                            writes=[gcf.res])
                self.cp("dve", gcb.t[:], gcf.t[:], [gcf.res], [gcb.res])
                for j in range(DC):
                    a = acc.next()
                    for br in range(4):
                        pb = self.load_panel(wp['br'], br * DC + j, BC)
                        pg = self.load_panel(wp['gu'], br * DC + j, 2)
                        psb = self.pring.next()
                        self.gemm_F(psb.t[:, :], psb.res, pb, BC, 128, hid, koff=br * BC)
                        psg = self.pring.next()
                        self.gemm_F(psg.t[:, :], psg.res, pg, 2, 128, gcb)
                        g = sm.next()
                        self.actv(g.t[:], psg.t[:], AF.Sigmoid, [psg.res, self.bgate.res], [g.res],
                                  bias=self.bgate.t[:, br * DC + j:br * DC + j + 1])
                        if br == 0:
                            self.tt("dve", a.t[:], g.t[:], psb.t[:], ALU.mult, [g.res, psb.res], [a.res])
                        else:
                            self.tt("dve", g.t[:], g.t[:], psb.t[:], ALU.mult, [g.res, psb.res], [g.res])
                            if br < 3:
                                self.tt("dve", a.t[:], a.t[:], g.t[:], ALU.add, [a.res, g.res], [a.res])
                            else:
                                self.tt("dve", actA.t[:, j, :], a.t[:], g.t[:], ALU.add, [a.res, g.res], [actA.res])
                for j in range(DC):
                    pw = self.load_panel(wp['out'], j, DC)
                    ps = self.pring.next()
                    self.gemm_F(ps.t[:, :], ps.res, pw, DC, 128, actA)
                    yt = sm.next()
                    self.evac(yt.t[:], ps.t[:], [ps.res], [yt.res])
                    self.fw.dma("pool", self.ybuf[j * 128:(j + 1) * 128, ts_], yt.t[:], reads=[yt.res], writes=[yres[j]])
                    sq = sm.next()
                    self.sumsq_accum(yt.t[:], yt.res, j, DC, sq)
                self.rstd_from_pss(rstd)
                self.residual_update(sm, rstd, ts_, G1, yres, xres, stats=True)
                self.rstd_from_pss(rstd)
                for j in range(DC):
                    t = sm.next()
                    self.fw.dma("pool", t.t[:], self.xT[j * 128:(j + 1) * 128, ts_], reads=[xres[j]], writes=[t.res])
                    self.stt(actA.t[:, j, :], t.t[:], self.gains.t[:, G2 + j:G2 + j + 1], rstd.t[:], ALU.mult, ALU.mult,
                             [t.res, self.gains.res, rstd.res], [actA.res])
                for j in range(FC):
                    pg = self.load_panel(wp['fg'], j, DC)
                    pu = self.load_panel(wp['fu'], j, DC)
                    psg = self.pring.next()
                    self.gemm_F(psg.t[:, :], psg.res, pg, DC, 128, actA)
                    psu = self.pring.next()
                    self.gemm_F(psu.t[:, :], psu.res, pu, DC, 128, actA)
                    g = sm.next()
                    self.actv(g.t[:], psg.t[:], AF.Silu, [psg.res], [g.res])
                    self.tt("dve", hid.t[:, j, :], g.t[:], psu.t[:], ALU.mult, [g.res, psu.res], [hid.res])
                for j in range(DC):
                    pw = self.load_panel(wp['fd'], j, FC)
                    ps = self.pring.next()
                    self.gemm_F(ps.t[:, :], ps.res, pw, FC, 128, hid)
                    yt = sm.next()
                    self.evac(yt.t[:], ps.t[:], [ps.res], [yt.res])
                    self.fw.dma("pool", self.ybuf[j * 128:(j + 1) * 128, ts_], yt.t[:], reads=[yt.res], writes=[yres[j]])
                    sq = sm.next()
                    self.sumsq_accum(yt.t[:], yt.res, j, DC, sq)
                self.rstd_from_pss(rstd)
                self.residual_update(sm, rstd, ts_, G3, yres, xres, stats=False)

    def residual_update(self, sm, rstd, ts_, gbase, yres, xres, stats):
        c = self.c
        for j in range(c.DC):
            yt = sm.next()
            self.fw.dma("pool", yt.t[:], self.ybuf[j * 128:(j + 1) * 128, ts_], reads=[yres[j]], writes=[yt.res])
            xt = sm.next()
            self.fw.dma("pool", xt.t[:], self.xT[j * 128:(j + 1) * 128, ts_], reads=[xres[j]], writes=[xt.res])
            self.stt(yt.t[:], yt.t[:], self.gains.t[:, gbase + j:gbase + j + 1], rstd.t[:], ALU.mult, ALU.mult,
                     [yt.res, self.gains.res, rstd.res], [yt.res])
            self.tt("dve", xt.t[:], xt.t[:], yt.t[:], ALU.add, [xt.res, yt.res], [xt.res])
            self.fw.dma("pool", self.xT[j * 128:(j + 1) * 128, ts_], xt.t[:], reads=[xt.res], writes=[xres[j]])
            if stats:
                sq = sm.next()
                self.sumsq_accum(xt.t[:], xt.res, j, c.DC, sq)

    def dbg_tile(self, name, ap, res, dt=F32):
        if name not in self.debug:
            return
        o = self.nc.dram_tensor("dbg_" + name, list(ap.shape), dt, kind="ExternalOutput").ap()
        r = Res()
        self.fw.dma("sp", o, ap, reads=[res], writes=[r])
        self.final_res.append(r)

    def dump(self, name, ap, dt=F32):
        self.fw.barrier()
        o = self.nc.dram_tensor(name, list(ap.shape), dt, kind="ExternalOutput").ap()
        r = Res()
        self.fw.dma("sp", o, ap, writes=[r])
        self.final_res.append(r)

    def build(self, stop_after=None):
        c = self.c
        self.final_res = []
        self.setup_consts()
        self.transpose_in()
        for l in range(c.L):
            self.load_layer_params(l)
            self.convert_layer(l)
            self.phase_norm_proj(l)
            if stop_after == ("proj", l):
                self.dump("dbg_projF", self.projF)
                self.dump("dbg_projT", self.projT)
                break
            self.phase_mixers(l)
            if stop_after == ("mix", l):
                self.dump("dbg_outsT", self.outsT, BF16)
                break
            self.phase_merge_ffn(l)
            if stop_after == ("layer", l):
                self.dump("dbg_xT", self.xT)
                break
        else:
            self.transpose_out()
            self.final_res.append(self.out_res)
        self.fw.barrier()
        for r in self.final_res:
            self.fw._need("sp", r.w)
        self.fw.emit()
        return self.nc


def make_inputs(cfg, inputs, carr):
    m = {"x": np.ascontiguousarray(np.asarray(inputs["x"], dtype=np.float32).reshape(cfg.T, cfg.D)),
         "consts": carr}
    for n in WEIGHT_NAMES:
        m[n] = np.ascontiguousarray(np.asarray(inputs[n], dtype=np.float32))
    return m


def kernel(**inputs):
    x = np.asarray(inputs["x"])
    B, T, D = x.shape
    L = np.asarray(inputs["w_in"]).shape[0]
    cfg = Cfg(D, T, L)
    b = Builder(cfg)
    nc = b.build()
    in_map = make_inputs(cfg, inputs, b.carr)
    res = run_bass_kernel_spmd(nc, [in_map], core_ids=[0])
    y = np.asarray(res.results[0]["y"], dtype=np.float32).reshape(B, T, D)
    return y


class TView:
    __slots__ = ("t", "res")

    def __init__(self, ap, res=None):
        self.t = ap
        self.res = res if res is not None else Res()


def _phase_mixers(self, l):
    c = self.c
    HG, AQ, AKV, SH, SG, REP, RH, BW, BC, XC = c.HG, c.AQ, c.AKV, c.SH, c.SG, c.REP, c.RH, c.BW, c.BC, c.XC
    NT_ = c.T // 128
    cst = self.cst
    U, M1, NEGM, ONES, IDT = cst['U'], cst['M1'], cst['NEGM'], cst['ones'], cst['ident']
    IDB = self.identb
    with self.scope() as S:
        def cload(key, rows=128):
            o, n = self.coff[key]
            t = S("k_" + key, [128, n], F32)
            self.fw.dma("sp", t.t[:], self.consts_d[:, o:o + n], writes=[t.res])
            return t
        ret_mt, ret_qd, ret_kd = cload('ret_mt'), cload('ret_qd'), cload('ret_kd')
        andist, pmask = cload('attn_nd'), cload('prevmask')
        slopes = [float(2.0 ** (-8.0 * (h + 1) / AQ)) for h in range(AQ)]
        hgn_bc = S("hgn_bc", [128, BW], F32)
        ssdn_bc = S("ssdn_bc", [128, BW], F32)
        dtb_bc = S("dtb_bc", [128, SH], F32)
        negA = S("negA", [128, SH], F32)
        dsk_bc = S("dsk_bc", [128, SH], F32)
        sink_bc = S("sink_bc", [128, AQ], F32)
        self.bcast_load("sp", hgn_bc, self.w['hgrn_norm'][l], BW)
        self.bcast_load("sp", ssdn_bc, self.w['ssd_norm'][l], BW)
        self.bcast_load("sp", dtb_bc, self.w['ssd_dt_bias'][l], SH)
        self.bcast_load("sp", negA, self.w['ssd_a_log'][l], SH)
        self.bcast_load("sp", dsk_bc, self.w['ssd_d'][l], SH)
        self.bcast_load("sp", sink_bc, self.w['attn_sinks'][l], AQ)
        self.actv(negA.t[:], negA.t[:], AF.Exp, [negA.res], [negA.res])
        self.ts("dve", negA.t[:], negA.t[:], -1.0, None, ALU.mult, None, [negA.res], [negA.res])
        convw = S("convw", [128, 4 * XC], F32)
        convb = S("convb", [128, XC], F32)
        self.load_cols(S, convw.t[:, :], convw.res, self.w['ssd_conv_w'][l].rearrange("j (a p) -> (j a) p", p=128), 4 * XC)
        self.load_cols(S, convb.t[:, :], convb.res, self.w['ssd_conv_b'][l].rearrange("(a p) -> a p", p=128), XC)
        hS = S("hS", [128, HG * 128], F32); hSb = S("hSb", [128, HG * 128], BF16)
        sS = S("sS", [128, SH * 64], F32); sSb = S("sSb", [128, SH * 64], BF16)
        rS = S("rS", [64, RH * 128], F32); rSb = S("rSb", [64, RH * 128], BF16)
        for t in (hS, hSb, sS, sSb, rS, rSb):
            self.memset("pool", t.t[:], 0.0, [t.res])
        kbuf = [S(f"kbuf{i}", [64, AKV, 128], BF16) for i in range(2)]
        vbuf = [S(f"vbuf{i}", [128, AKV, 64], BF16) for i in range(2)]
        for t in kbuf + vbuf:
            self.memset("pool", t.t[:], 0.0, [t.res])
        xh = S("xh", [128, XC, 131], F32)
        self.memset("pool", xh.t[:], 0.0, [xh.res])
        ktil = [S(f"ktil{j}", [128, 128], BF16) for j in range(4)]
        for t in ktil:
            self.memset("pool", t.t[:], 0.0, [t.res])
        bpad = S("bpad", [128, HG, 129], F32)
        self.memset("pool", bpad.t[:], 0.0, [bpad.res])
        rj = S("rj", [128, HG, 4, 1], F32)
        fh = Ring([S(f"fh{i}", [128, 2 * HG, 128], F32) for i in range(1)])
        tin = Ring([S(f"tin{i}", [128, c.NT], F32) for i in range(1)])
        faq = S("faq", [64, AQ, 128], F32)
        fak = S("fak", [64, AKV, 128], F32)
        fr = S("fr", [64, 2 * RH, 128], F32)
        w32 = Ring([S(f"w32_{i}", [128, max(HG, 4) * 128], F32) for i in range(4)])
        w16 = Ring([S(f"w16_{i}", [128, max(HG, 4) * 128], BF16) for i in range(4)])
        sm32 = Ring([S(f"sm32_{i}", [128, 128], F32) for i in range(6)])
        sm16 = Ring([S(f"sm16_{i}", [128, 256], BF16) for i in range(6)])
        tiny = Ring([S(f"tiny{i}", [128, 32], F32) for i in range(12)])
        outT = Ring([S(f"outT{i}", [128, BW], BF16) for i in range(2)])
        obT = Ring([S(f"obT{i}", [128, BC, 128], BF16) for i in range(2)])
        o32 = S("o32", [128, BW], F32)
        cacc = S("cacc", [128, XC, 128], F32)
        xc = cacc
        xsT = S("xsT", [128, BW], F32)
        bcb = S("bcb", [128, 8, 128], BF16)
        btm = S("btm", [128, 4, 128], BF16)
        v32 = S("v32", [128, BW], F32)
        vb16 = S("vb16", [128, BW], BF16)
        vdec = S("vdec", [128, BW], BF16)
        yss = S("yss", [128, BW], F32)
        hib = S("hib", [128, BW], BF16)
        rvb = S("rvb", [128, BW], BF16)
        t2 = S("t2", [128, BW], F32)
        ded = {n: S("d_" + n, [128, 32], F32) for n in ("ssq", "a_", "ebt", "edec", "mxa", "rsum", "s1", "s2", "dt")}
        halo = S("halo", [128, XC, 3], F32)
        rqb = S("rqb", [64, 2 * RH * 128], BF16)
        aqb = S("aqb", [64, AQ * 128], BF16)
        pt = self.pring.tiles
        pshort = Ring([pt[0], pt[1], pt[2], self.pss])
        plong = Ring([pt[3], pt[4], pt[5]])
        ptb = [TView(self.ptb.t[:, 0:512], self.ptb.res), TView(self.ptb.t[:, 512:1024], self.ptb.res)]
        ptbi = [0]

        def ptb_next():
            ptbi[0] ^= 1
            return ptb[ptbi[0]]

        def emit_outs(br, i, ot):
            ob = obT.next()
            for cc in range(BC):
                pv = ptb[(cc * 128) // 512]
                col = (cc * 128) % 512
                self.tr(pv.t[:, col:col + 128], ot.t[:, cc * 128:(cc + 1) * 128], IDB.t[:], [ot.res, IDB.res], [pv.res])
            for half in range((BC * 128 + 511) // 512):
                n = min(512, BC * 128 - half * 512)
                self.evac(ob.t[:, half * 4:half * 4 + n // 128, :].rearrange("p a b -> p (a b)"), ptb[half].t[:, 0:n],
                          [ptb[half].res], [ob.res])
            dst = self.outsT[br * BW:(br + 1) * BW, i * 128:(i + 1) * 128].rearrange("(a p) t -> p a t", p=128)
            self.fw.dma("pool", dst, ob.t[:], reads=[ob.res])

        def rsqrt_small(ap, res, scale, eps):
            self.ts("dve", ap, ap, scale, eps, ALU.mult, ALU.add, [res], [res])
            self.actv(ap, ap, AF.Sqrt, [res], [res])
            self.fw.op("dve", lambda h: h.reciprocal(out=ap, in_=ap), [res], [res])

        for i in range(NT_):
            tsl = slice(i * 128, (i + 1) * 128)
            f_h = fh.next()
            self.fw.dma("sp", f_h.t[:], self.projF[c.foff['hq']:c.foff['hq'] + 2 * BW, tsl].rearrange("(a p) t -> p a t", p=128),
                        writes=[f_h.res])
            tt_ = tin.next()
            self.fw.dma("sp", tt_.t[:], self.projT[tsl, :], writes=[tt_.res])
            self.fw.dma("sp", faq.t[:], self.projF[c.foff['aq']:c.foff['aq'] + AQ * 64, tsl].rearrange("(h d) t -> d h t", d=64),
                        writes=[faq.res])
            self.fw.dma("sp", fak.t[:], self.projF[c.foff['ak']:c.foff['ak'] + AKV * 64, tsl].rearrange("(h d) t -> d h t", d=64),
                        writes=[fak.res])
            self.fw.dma("sp", fr.t[:], self.projF[c.foff['rq']:c.foff['rq'] + 2 * RH * 64, tsl].rearrange("(h d) t -> d h t", d=64),
                        writes=[fr.res])
            self.fw.dma("sp", xh.t[:, :, 3:131], self.projF[c.foff['sxbc']:c.foff['sxbc'] + c.XBC, tsl].rearrange("(a p) t -> p a t", p=128),
                        writes=[xh.res])
            T = tt_.t

            def tcols(name, k0=0, n=None):
                o = c.toff[name] + k0
                return T[:, o:o + (c.size[name] - k0 if n is None else n)]

            N1 = HG * 128
            q_ap = f_h.t[:, 0:HG, :].rearrange("p a b -> p (a b)")
            f_ap = f_h.t[:, HG:2 * HG, :].rearrange("p a b -> p (a b)")
            fg = w32.next(); al = w32.next(); kk = w32.next(); qs = w32.next()
            self.actv(fg.t[:, 0:N1], f_ap, AF.Sigmoid, [f_h.res], [fg.res])
            for h in range(HG):
                hs = slice(h * 128, (h + 1) * 128)
                self.ts("dve", fg.t[:, hs], fg.t[:, hs], self.oml.t[:, l * HG + h:l * HG + h + 1],
                        self.lb.t[:, l * HG + h:l * HG + h + 1], ALU.mult, ALU.add, [fg.res, self.oml.res, self.lb.res], [fg.res])
            self.actv(al.t[:, 0:N1], fg.t[:, 0:N1], AF.Ln, [fg.res], [al.res])
            self.ts("pool", kk.t[:, 0:N1], fg.t[:, 0:N1], -1.0, 1.0, ALU.mult, ALU.add, [fg.res], [kk.res])
            self.actv(qs.t[:, 0:N1], q_ap, AF.Silu, [f_h.res], [qs.res])
            for h in range(HG):
                hs = slice(h * 128, (h + 1) * 128)
                self.scan(bpad.t[:, h, 1:129], ONES.t[:, :], al.t[:, hs], [al.res, ONES.res], [bpad.res])
                self.cp("pool", rj.t[:, h, :, 0], bpad.t[:, h, 0:128:32], [bpad.res], [rj.res])
            bm = w32.next(); eb = w32.next()
            for h in range(HG):
                hs = slice(h * 128, (h + 1) * 128)
                self.tt("dve", bm.t[:, hs].rearrange("p (j t) -> p j t", t=32),
                        bpad.t[:, h, 1:129].rearrange("p (j t) -> p j t", t=32),
                        rj.t[:, h, :, :].broadcast_to([128, 4, 32]), ALU.subtract, [bpad.res, rj.res], [bm.res])
            self.actv(bm.t[:, 0:N1], bm.t[:, 0:N1], AF.Exp, [bm.res], [bm.res])
            self.actv(eb.t[:, 0:N1].rearrange("p (a b) -> p a b", b=128), bpad.t[:, :, 1:129], AF.Exp, [bpad.res], [eb.res])
            qt = w16.next(); qb = w16.next()
            self.tt("dve", qt.t[:, 0:N1], qs.t[:, 0:N1], bm.t[:, 0:N1], ALU.mult, [qs.res, bm.res], [qt.res])
            self.tt("pool", qb.t[:, 0:N1], qs.t[:, 0:N1], eb.t[:, 0:N1], ALU.mult, [qs.res, eb.res], [qb.res])
            self.cp("pool", hib.t[:], tcols('hi'), [tt_.res], [hib.res])
            self.actv(t2.t[:], tcols('hg'), AF.Silu, [tt_.res], [t2.res])
            self.tt("dve", t2.t[:], t2.t[:], hgn_bc.t[:], ALU.mult, [t2.res, hgn_bc.res], [t2.res])
            ssq = ded["ssq"]
            for h in range(HG):
                hs = slice(h * 128, (h + 1) * 128)
                for J in range(4):
                    wd = 32 * (J + 1)
                    tmp = sm32.next()
                    self.actv(tmp.t[:, 0:wd], bpad.t[:, h, 1:1 + wd], AF.Exp, [bpad.res], [tmp.res],
                              bias=bpad.t[:, h, 32 * J:32 * J + 1], scale=-1.0)
                    self.tt("dve" if J % 2 else "pool", ktil[J].t[:, 0:wd], kk.t[:, h * 128:h * 128 + wd], tmp.t[:, 0:wd], ALU.mult,
                            [kk.res, tmp.res], [ktil[J].res])
                tmp = sm32.next()
                self.actv(tmp.t[:], bpad.t[:, h, 1:129], AF.Exp, [bpad.res], [tmp.res], bias=bpad.t[:, h, 128:129], scale=-1.0)
                kdT = sm16.next()
                self.tt("dve", kdT.t[:, 0:128], kk.t[:, hs], tmp.t[:], ALU.mult, [kk.res, tmp.res], [kdT.res])
                pv = ptb_next()
                self.tr(pv.t[:, 0:128], kdT.t[:, 0:128], IDB.t[:], [kdT.res, IDB.res], [pv.res])
                kd = sm16.next()
                self.evac(kd.t[:, 0:128], pv.t[:, 0:128], [pv.res], [kd.res])
                ebl = tiny.next()
                self.actv(ebl.t[:, 0:1], bpad.t[:, h, 128:129], AF.Exp, [bpad.res], [ebl.res])
                psT = pshort.next()
                for J in range(4):
                    self.mm(psT.t[:, 32 * J:32 * J + 32], ktil[J].t[:, :], qt.t[:, h * 128 + 32 * J:h * 128 + 32 * J + 32], True, True,
                            [ktil[J].res, qt.res], [psT.res])
                PT = sm16.next()
                self.tt("dve", PT.t[:, 0:128], psT.t[:, 0:128], U.t[:], ALU.mult, [psT.res, U.res], [PT.res])
                po = pshort.next()
                self.mm(po.t[:, 0:128], PT.t[:, 0:128], hib.t[:, hs], True, False, [PT.res, hib.res], [po.res])
                self.mm(po.t[:, 0:128], qb.t[:, hs], hSb.t[:, hs], False, True, [qb.res, hSb.res], [po.res])
                pS_ = pshort.next()
                self.mm(pS_.t[:, 0:128], kd.t[:, 0:128], hib.t[:, hs], True, True, [kd.res, hib.res], [pS_.res])
                self.stt(hS.t[:, hs], hS.t[:, hs], ebl.t[:, 0:1], pS_.t[:, 0:128], ALU.mult, ALU.add, [hS.res, ebl.res, pS_.res], [hS.res])
                self.cp("pool", hSb.t[:, hs], hS.t[:, hs], [hS.res], [hSb.res])
                junk = sm32.next()
                self.actv(junk.t[:], po.t[:, 0:128], AF.Square, [po.res], [junk.res, ssq.res], accum=ssq.t[:, h:h + 1])
                self.cp("dve", o32.t[:, hs], po.t[:, 0:128], [po.res], [o32.res])
            rsqrt_small(ssq.t[:, 0:HG], ssq.res, 1.0 / 128, 1e-6)
            ot = outT.next()
            for h in range(HG):
                hs = slice(h * 128, (h + 1) * 128)
                self.stt(ot.t[:, hs], o32.t[:, hs], ssq.t[:, h:h + 1], t2.t[:, hs], ALU.mult, ALU.mult, [o32.res, ssq.res, t2.res], [ot.res])
            emit_outs(0, i, ot)

            self.cp("pool", rqb.t[:, :].rearrange("p (a b) -> p a b", b=128), fr.t[:, :, :], [fr.res], [rqb.res])
            self.cp("pool", rvb.t[:], tcols('rv'), [tt_.res], [rvb.res])
            self.actv(t2.t[:], tcols('rg'), AF.Silu, [tt_.res], [t2.res])
            s1, s2 = ded["s1"], ded["s2"]
            for h in range(RH):
                hs = slice(h * 128, (h + 1) * 128)
                psT = pshort.next()
                self.mm(psT.t[:, 0:128], rqb.t[0:64, (RH + h) * 128:(RH + h + 1) * 128], rqb.t[0:64, hs], True, True, [rqb.res], [psT.res])
                PT = sm16.next()
                self.tt("dve", PT.t[:, 0:128], psT.t[:, 0:128], ret_mt.t[:, hs], ALU.mult, [psT.res, ret_mt.res], [PT.res])
                qd = sm16.next()
                self.tt("pool", qd.t[0:64, 0:128], fr.t[:, h, :], ret_qd.t[0:64, hs], ALU.mult, [fr.res, ret_qd.res], [qd.res])
                po = pshort.next()
                self.mm(po.t[:, 0:128], PT.t[:, 0:128], rvb.t[:, hs], True, False, [PT.res, rvb.res], [po.res])
                self.mm(po.t[:, 0:128], qd.t[0:64, 0:128], rSb.t[:, hs], False, True, [qd.res, rSb.res], [po.res])
                kdc = sm16.next()
                self.ts("pool", kdc.t[:, 0:64], tcols('rk', h * 64, 64), ret_kd.t[:, h:h + 1], None, ALU.mult, None,
                        [tt_.res, ret_kd.res], [kdc.res])
                pS_ = pshort.next()
                self.mm(pS_.t[0:64, 0:128], kdc.t[:, 0:64], rvb.t[:, hs], True, True, [kdc.res, rvb.res], [pS_.res])
                self.stt(rS.t[:, hs], rS.t[:, hs], self.gam128[h], pS_.t[0:64, 0:128], ALU.mult, ALU.add, [rS.res, pS_.res], [rS.res])
                self.cp("pool", rSb.t[:, hs], rS.t[:, hs], [rS.res], [rSb.res])
                junk = sm32.next()
                self.actv(junk.t[:], po.t[:, 0:128], AF.Square, [po.res], [junk.res, s2.res], accum=s2.t[:, h:h + 1])
                self.actv(o32.t[:, hs], po.t[:, 0:128], AF.Copy, [po.res], [o32.res, s1.res], accum=s1.t[:, h:h + 1])
            mean = tiny.next(); rstd = tiny.next()
            self.ts("dve", mean.t[:, 0:RH], s1.t[:, 0:RH], 1.0 / 128, None, ALU.mult, None, [s1.res], [mean.res])
            self.tt("dve", rstd.t[:, 0:RH], mean.t[:, 0:RH], mean.t[:, 0:RH], ALU.mult, [mean.res], [rstd.res])
            self.stt(rstd.t[:, 0:RH], s2.t[:, 0:RH], 1.0 / 128, rstd.t[:, 0:RH], ALU.mult, ALU.subtract, [s2.res, rstd.res], [rstd.res])
            rsqrt_small(rstd.t[:, 0:RH], rstd.res, 1.0, 1e-5)
            ot = outT.next()
            for h in range(RH):
                hs = slice(h * 128, (h + 1) * 128)
                self.ts("dve", o32.t[:, hs], o32.t[:, hs], mean.t[:, h:h + 1], rstd.t[:, h:h + 1], ALU.subtract, ALU.mult,
                        [o32.res, mean.res, rstd.res], [o32.res])
                self.tt("dve", ot.t[:, hs], o32.t[:, hs], t2.t[:, hs], ALU.mult, [o32.res, t2.res], [ot.res])
            emit_outs(3, i, ot)

            for cc in range(XC):
                self.ts("dve", cacc.t[:, cc, :], xh.t[:, cc, 0:128], convw.t[:, cc:cc + 1], None, ALU.mult, None,
                        [xh.res, convw.res], [cacc.res])
                for j in range(1, 4):
                    self.stt(cacc.t[:, cc, :], xh.t[:, cc, j:j + 128], convw.t[:, j * XC + cc:j * XC + cc + 1], cacc.t[:, cc, :],
                             ALU.mult, ALU.add, [xh.res, convw.res, cacc.res], [cacc.res])
                self.actv(xc.t[:, cc, :], cacc.t[:, cc, :], AF.Silu, [cacc.res, convb.res], [xc.res], bias=convb.t[:, cc:cc + 1])
            self.cp("pool", halo.t[:], xh.t[:, :, 128:131], [xh.res], [halo.res])
            self.cp("pool", xh.t[:, :, 0:3], halo.t[:], [halo.res], [xh.res])
            for g4 in range((BC + 3) // 4):
                ps = pshort.next()
                n4 = min(4, BC - g4 * 4)
                for k in range(n4):
                    self.tr(ps.t[:, k * 128:(k + 1) * 128], xc.t[:, g4 * 4 + k, :], IDT.t[:], [xc.res, IDT.res], [ps.res])
                self.evac(xsT.t[:, g4 * 512:g4 * 512 + n4 * 128], ps.t[:, 0:n4 * 128], [ps.res], [xsT.res])
            self.cp("pool", bcb.t[:], xc.t[:, BC:BC + 8, :], [xc.res], [bcb.res])
            pv = ptb_next()
            for g in range(4):
                self.tr(pv.t[:, g * 128:(g + 1) * 128], bcb.t[:, g, :], IDB.t[:], [bcb.res, IDB.res], [pv.res])
            self.evac(btm.t[:].rearrange("p a b -> p (a b)"), pv.t[:, 0:512], [pv.res], [btm.res])
            u = tiny.next(); ab = tiny.next(); dt = ded["dt"]; a_ = ded["a_"]
            self.tt("dve", u.t[:, 0:SH], tcols('sdt'), dtb_bc.t[:], ALU.add, [tt_.res, dtb_bc.res], [u.res])
            self.ts("dve", ab.t[:, 0:SH], u.t[:, 0:SH], -1.0, None, ALU.mult, None, [u.res], [ab.res])
            self.tt("dve", ab.t[:, 0:SH], ab.t[:, 0:SH], u.t[:, 0:SH], ALU.max, [ab.res, u.res], [ab.res])
            self.actv(ab.t[:, 0:SH], ab.t[:, 0:SH], AF.Exp, [ab.res], [ab.res], scale=-1.0)
            self.actv(ab.t[:, 0:SH], ab.t[:, 0:SH], AF.Ln, [ab.res, ONES.res], [ab.res], bias=ONES.t[:, 0:1])
            self.stt(dt.t[:, 0:SH], u.t[:, 0:SH], 0.0, ab.t[:, 0:SH], ALU.max, ALU.add, [u.res, ab.res], [dt.res])
            self.tt("dve", a_.t[:, 0:SH], dt.t[:, 0:SH], negA.t[:], ALU.mult, [dt.res, negA.res], [a_.res])
            pcs = pshort.next()
            self.mm(pcs.t[:, 0:SH], U.t[:], a_.t[:, 0:SH], True, True, [U.res, a_.res], [pcs.res])
            self.mm(pcs.t[:, 32:32 + SH], ONES.t[:], a_.t[:, 0:SH], True, True, [ONES.res, a_.res], [pcs.res])
            bsb = tiny.next(); ebt = ded["ebt"]; edec = ded["edec"]; wdec = tiny.next()
            self.cp("dve", bsb.t[:, 0:SH], pcs.t[:, 0:SH], [pcs.res], [bsb.res])
            self.actv(ebt.t[:, 0:SH], pcs.t[:, 0:SH], AF.Exp, [pcs.res], [ebt.res])
            self.actv(edec.t[:, 0:SH], pcs.t[:, 32:32 + SH], AF.Exp, [pcs.res], [edec.res])
            self.tt("dve", wdec.t[:, 0:SH], pcs.t[:, 32:32 + SH], bsb.t[:, 0:SH], ALU.subtract, [pcs.res, bsb.res], [wdec.res])
            self.actv(wdec.t[:, 0:SH], wdec.t[:, 0:SH], AF.Exp, [wdec.res], [wdec.res])
            v3 = lambda t_: t_.t[:].rearrange("p (h d) -> p h d", d=64)
            b3 = lambda t_: t_.t[:, 0:SH].rearrange("p (h o) -> p h o", o=1).broadcast_to([128, SH, 64])
            self.tt("dve", v3(v32), v3(xsT), b3(dt), ALU.mult, [xsT.res, dt.res], [v32.res])
            self.cp("pool", vb16.t[:], v32.t[:], [v32.res], [vb16.res])
            self.tt("pool", v3(vdec), v3(v32), b3(wdec), ALU.mult, [v32.res, wdec.res], [vdec.res])
            NH = max(1, BW // 512)
            GPH = SG // NH
            for hh in range(NH):
                p_in = plong.next(); p_x = plong.next(); p_S = plong.next()
                for gg in range(GPH):
                    g = hh * GPH + gg
                    gw = REP * 64
                    gc0 = gg * gw
                    gs0 = g * gw
                    psc = pshort.next()
                    self.mm(psc.t[:, 0:128], bcb.t[:, g, :], bcb.t[:, 4 + g, :], True, True, [bcb.res], [psc.res])
                    psD = pshort.next()
                    for r in range(REP):
                        h = g * REP + r
                        aU = sm32.next()
                        self.ts("pool", aU.t[:], U.t[:], a_.t[:, h:h + 1], None, ALU.mult, None, [U.res, a_.res], [aU.res])
                        self.mm(psD.t[:, r * 128:(r + 1) * 128], M1.t[:], aU.t[:], True, False, [M1.res, aU.res], [psD.res])
                        self.mm(psD.t[:, r * 128:(r + 1) * 128], IDT.t[:], NEGM.t[:], False, True, [IDT.res, NEGM.res], [psD.res])
                    E = w32.next()
                    self.actv(E.t[:, 0:REP * 128], psD.t[:, 0:REP * 128], AF.Exp, [psD.res], [E.res])
                    PTg = w16.next()
                    self.tt("dve", PTg.t[:, 0:REP * 128].rearrange("p (r t) -> p r t", t=128),
                            E.t[:, 0:REP * 128].rearrange("p (r t) -> p r t", t=128),
                            psc.t[:, 0:128].rearrange("p (o t) -> p o t", o=1).broadcast_to([128, REP, 128]), ALU.mult,
                            [E.res, psc.res], [PTg.res])
                    for r in range(REP):
                        h = g * REP + r
                        self.mm(p_in.t[:, gc0 + r * 64:gc0 + (r + 1) * 64], PTg.t[:, r * 128:(r + 1) * 128], vb16.t[:, h * 64:(h + 1) * 64],
                                True, True, [PTg.res, vb16.res], [p_in.res])
                    self.mm(p_x.t[:, gc0:gc0 + gw], bcb.t[:, 4 + g, :], sSb.t[:, gs0:gs0 + gw], True, True, [bcb.res, sSb.res], [p_x.res])
                    self.mm(p_S.t[:, gc0:gc0 + gw], btm.t[:, g, :], vdec.t[:, gs0:gs0 + gw], True, True, [btm.res, vdec.res], [p_S.res])
                hw = GPH * REP * 64
                h0 = hh * GPH * REP
                hsl = slice(hh * hw, (hh + 1) * hw)
                nh = GPH * REP
                v3h = lambda ap: ap.rearrange("p (h d) -> p h d", d=64)
                bh = lambda t_: t_.t[:, h0:h0 + nh].rearrange("p (h o) -> p h o", o=1).broadcast_to([128, nh, 64])
                self.tt("dve", v3h(yss.t[:, hsl]), v3h(p_x.t[:, 0:hw]), bh(ebt), ALU.mult, [p_x.res, ebt.res], [yss.res])
                self.tt("dve", yss.t[:, hsl], yss.t[:, hsl], p_in.t[:, 0:hw], ALU.add, [yss.res, p_in.res], [yss.res])
                self.tt("pool", v3h(v32.t[:, hsl]), v3h(xsT.t[:, hsl]), bh(dsk_bc), ALU.mult, [xsT.res, dsk_bc.res], [v32.res])
                self.tt("dve", yss.t[:, hsl], yss.t[:, hsl], v32.t[:, hsl], ALU.add, [yss.res, v32.res], [yss.res])
                self.tt("dve", v3h(sS.t[:, hsl]), v3h(sS.t[:, hsl]), bh(edec), ALU.mult, [sS.res, edec.res], [sS.res])
                self.tt("dve", sS.t[:, hsl], sS.t[:, hsl], p_S.t[:, 0:hw], ALU.add, [sS.res, p_S.res], [sS.res])
                self.cp("pool", sSb.t[:, hsl], sS.t[:, hsl], [sS.res], [sSb.res])
            self.actv(t2.t[:], tcols('sz'), AF.Silu, [tt_.res], [t2.res])
            self.tt("dve", yss.t[:], yss.t[:], t2.t[:], ALU.mult, [yss.res, t2.res], [yss.res])
            sss = tiny.next()
            self.actv(t2.t[:], yss.t[:], AF.Square, [yss.res], [t2.res, sss.res], accum=sss.t[:, 0:1])
            rsqrt_small(sss.t[:, 0:1], sss.res, 1.0 / BW, 1e-6)
            ot = outT.next()
            self.stt(ot.t[:], yss.t[:], sss.t[:, 0:1], ssdn_bc.t[:], ALU.mult, ALU.mult, [yss.res, sss.res, ssdn_bc.res], [ot.res])
            emit_outs(2, i, ot)

            cur, prv = i % 2, 1 - (i % 2)
            kb_, vb_ = kbuf[cur], vbuf[cur]
            self.cp("pool", kb_.t[:], fak.t[:], [fak.res], [kb_.res])
            self.cp("pool", vb_.t[:], tcols('av').rearrange("p (h d) -> p h d", d=64), [tt_.res], [vb_.res])
            self.ts("pool", aqb.t[:, :].rearrange("p (a b) -> p a b", b=128), faq.t[:, :, :], 0.125, None, ALU.mult, None,
                    [faq.res], [aqb.res])
            mxa = ded["mxa"]; rsum = ded["rsum"]
            NA8 = (AQ + 7) // 8
            for a8 in range(NA8):
                po = plong.next()
                nh8 = min(8, AQ - a8 * 8)
                for pr in range(nh8 // 2):
                    h0 = a8 * 8 + pr * 2
                    ps = pshort.next()
                    for k in range(2):
                        h = h0 + k
                        g = h // 4
                        self.mm(ps.t[:, k * 256:k * 256 + 128], aqb.t[0:64, h * 128:(h + 1) * 128], kbuf[prv].t[:, g, :], True, True,
                                [aqb.res, kbuf[prv].res], [ps.res])
                        self.mm(ps.t[:, k * 256 + 128:k * 256 + 256], aqb.t[0:64, h * 128:(h + 1) * 128], kb_.t[:, g, :], True, True,
                                [aqb.res, kb_.res], [ps.res])
                    sc = w32.next()
                    for k in range(2):
                        self.stt(sc.t[:, k * 256:(k + 1) * 256], andist.t[:, :], slopes[h0 + k], ps.t[:, k * 256:(k + 1) * 256],
                                 ALU.mult, ALU.add, [andist.res, ps.res], [sc.res])
                    if i == 0:
                        self.tt("dve", sc.t[:, 0:512].rearrange("p (k s) -> p k s", s=256), sc.t[:, 0:512].rearrange("p (k s) -> p k s", s=256),
                                pmask.t[:, :].rearrange("p (o s) -> p o s", o=1).broadcast_to([128, 2, 256]), ALU.add,
                                [sc.res, pmask.res], [sc.res])
                    self.red(mxa.t[:, h0:h0 + 2], sc.t[:, 0:512].rearrange("p (k s) -> p k s", s=256), ALU.max, [sc.res], [mxa.res])
                    self.tt("dve", mxa.t[:, h0:h0 + 2], mxa.t[:, h0:h0 + 2], sink_bc.t[:, h0:h0 + 2], ALU.max, [mxa.res, sink_bc.res], [mxa.res])
                    nmx = tiny.next()
                    self.ts("dve", nmx.t[:, 0:2], mxa.t[:, h0:h0 + 2], -1.0, None, ALU.mult, None, [mxa.res], [nmx.res])
                    for k in range(2):
                        h = h0 + k
                        g = h // 4
                        Pm = sm16.next()
                        self.actv(Pm.t[:, 0:256], sc.t[:, k * 256:(k + 1) * 256], AF.Exp, [sc.res, nmx.res], [Pm.res, rsum.res],
                                  bias=nmx.t[:, k:k + 1], accum=rsum.t[:, h:h + 1])
                        pv = ptb_next()
                        self.tr(pv.t[:, 0:128], Pm.t[:, 0:128], IDB.t[:], [Pm.res, IDB.res], [pv.res])
                        self.tr(pv.t[:, 128:256], Pm.t[:, 128:256], IDB.t[:], [Pm.res, IDB.res], [pv.res])
                        PTm = sm16.next()
                        self.evac(PTm.t[:, 0:256], pv.t[:, 0:256], [pv.res], [PTm.res])
                        hc = (h % 8) * 64
                        self.mm(po.t[:, hc:hc + 64], PTm.t[:, 0:128], vbuf[prv].t[:, g, :], True, False, [PTm.res, vbuf[prv].res], [po.res])
                        self.mm(po.t[:, hc:hc + 64], PTm.t[:, 128:256], vb_.t[:, g, :], False, True, [PTm.res, vb_.res], [po.res])
                hsl8 = slice(a8 * 8, a8 * 8 + nh8)
                es = tiny.next()
                self.tt("dve", es.t[:, 0:nh8], sink_bc.t[:, hsl8], mxa.t[:, hsl8], ALU.subtract, [sink_bc.res, mxa.res], [es.res])
                self.actv(es.t[:, 0:nh8], es.t[:, 0:nh8], AF.Exp, [es.res], [es.res])
                self.tt("dve", es.t[:, 0:nh8], es.t[:, 0:nh8], rsum.t[:, hsl8], ALU.add, [es.res, rsum.res], [es.res])
                self.recip(es.t[:, 0:nh8], es.t[:, 0:nh8], [es.res], [es.res])
                if a8 == 0:
                    ota = outT.next()
                self.tt("dve", ota.t[:, a8 * 512:a8 * 512 + nh8 * 64].rearrange("p (h d) -> p h d", d=64),
                        po.t[:, 0:nh8 * 64].rearrange("p (h d) -> p h d", d=64),
                        es.t[:, 0:nh8].rearrange("p (h o) -> p h o", o=1).broadcast_to([128, nh8, 64]), ALU.mult,
                        [po.res, es.res], [ota.res])
            emit_outs(1, i, ota)


Builder.phase_mixers = _phase_mixers
```
